# Optimizing a Trainium2 kernel written in Bass

```python
import math
import jax, jax.numpy as jnp
from jax import lax
import numpy as np

D_MODEL = 1024
BATCH = 8
SEQ = 2048
DEPTH = 4

GRID_W = 64
CTX_LEN = 256
MIX_WIDTH = D_MODEL
DN_WIDTH = MIX_WIDTH // 2
DN_HEADS = 4
DN_HEAD_DIM = DN_WIDTH // DN_HEADS
N_DIR = 2
CONV_K = 4
CHUNK = 64
POOL_WIDTH = MIX_WIDTH - DN_WIDTH
POOL_WINDOWS = (2, 4, 8, 16)
POOL_GROUPS = len(POOL_WINDOWS)
POOL_GROUP_DIM = POOL_WIDTH // POOL_GROUPS
N_EXPERTS = 16
EC_CAPACITY = 2
EXPERT_FF = D_MODEL
EPS = 1e-6
QKV_COLS = 3 * DN_WIDTH
GATE_COLS = N_DIR * DN_HEADS
STATE_COLS = QKV_COLS + 2 * GATE_COLS
IN_COLS = STATE_COLS + DN_WIDTH + POOL_WIDTH

kernel_name = 'hybrid_pool_gdn_ec_flow_trunk'


def rmsnorm(x, g):
    xf = x.astype(jnp.float32)
    y = xf * lax.rsqrt(jnp.mean(xf * xf, axis=-1, keepdims=True) + EPS)
    return (y * g.astype(jnp.float32)).astype(x.dtype)


def l2norm(x):
    xf = x.astype(jnp.float32)
    return xf * lax.rsqrt(jnp.sum(xf * xf, axis=-1, keepdims=True) + EPS)


def adaln_params(cond, w_mod, b_mod):
    return jnp.split(jax.nn.silu(cond) @ w_mod + b_mod, 6, axis=-1)


def modulate(h, shift, scale):
    return h * (1 + scale) + shift


def window_bounds(n, w):
    pos = jnp.arange(n)
    return jnp.clip(pos - w // 2, 0, n), jnp.clip(pos - w // 2 + w, 0, n)


def centred_box_mean(u, rows):
    B_, L, C = u.shape
    uf = u.astype(jnp.float32)
    outs = []
    if rows is None:
        s = jnp.pad(jnp.cumsum(uf, axis=1), ((0, 0), (1, 0), (0, 0)))
        for i, w in enumerate(POOL_WINDOWS):
            sg = s[..., i * POOL_GROUP_DIM:(i + 1) * POOL_GROUP_DIM]
            lo, hi = window_bounds(L, w)
            cnt = (hi - lo).astype(jnp.float32)
            outs.append((jnp.take(sg, hi, axis=1) - jnp.take(sg, lo, axis=1)) / cnt[None, :, None])
        return jnp.concatenate(outs, axis=-1)
    grid = uf.reshape(B_, rows, GRID_W, C)
    s = jnp.pad(jnp.cumsum(jnp.cumsum(grid, axis=1), axis=2), ((0, 0), (1, 0), (1, 0), (0, 0)))
    for i, w in enumerate(POOL_WINDOWS):
        sg = s[..., i * POOL_GROUP_DIM:(i + 1) * POOL_GROUP_DIM]
        lr, hr = window_bounds(rows, w)
        lc, hc = window_bounds(GRID_W, w)
        s_hi = jnp.take(sg, hr, axis=1)
        s_lo = jnp.take(sg, lr, axis=1)
        tot = (jnp.take(s_hi, hc, axis=2) - jnp.take(s_hi, lc, axis=2)
               - jnp.take(s_lo, hc, axis=2) + jnp.take(s_lo, lc, axis=2))
        cnt = ((hr - lr)[:, None] * (hc - lc)[None, :]).astype(jnp.float32)
        outs.append(tot / cnt[None, :, :, None])
    return jnp.concatenate(outs, axis=-1).reshape(B_, L, C)


def pool_mixer(u, pool_w, pool_scale, rows):
    B_, L, _ = u.shape
    m = (centred_box_mean(u, rows) - u.astype(jnp.float32)).astype(u.dtype)
    m = m.reshape(B_, L, POOL_GROUPS, POOL_GROUP_DIM)
    y = jnp.einsum('blgc,gcd->blgd', m, pool_w).reshape(B_, L, POOL_WIDTH)
    return y * pool_scale


def short_conv(x, w):
    C = x.shape[-1]
    return lax.conv_general_dilated(
        x, w[:, None, :], window_strides=(1,),
        padding=[(CONV_K // 2, CONV_K - 1 - CONV_K // 2)],
        dimension_numbers=('NWC', 'WIO', 'NWC'), feature_group_count=C)


def deltanet_inputs(feat_pre, ab_pre, conv_w, a_log, dt_bias):
    B_, L, _ = feat_pre.shape
    feats = jax.nn.silu(short_conv(feat_pre, conv_w))
    heads = lambda t: jnp.transpose(t.reshape(B_, L, DN_HEADS, DN_HEAD_DIM), (0, 2, 1, 3))
    parts = [heads(t) for t in jnp.split(feats, feats.shape[-1] // DN_WIDTH, axis=-1)]
    ab = ab_pre.astype(jnp.float32).reshape(B_, L, 2, N_DIR, DN_HEADS)
    log_a = -jnp.exp(a_log.astype(jnp.float32)) * jax.nn.softplus(ab[:, :, 0] + dt_bias.astype(jnp.float32))
    beta = jax.nn.sigmoid(ab[:, :, 1])
    to_dir = lambda t: jnp.transpose(t, (2, 0, 3, 1))
    return parts, to_dir(log_a), to_dir(beta)


def chunk_gated_delta(q, k, v, log_a, beta, s0):
    B_, H, L, _ = k.shape
    n = L // CHUNK
    f32 = jnp.float32
    blocks = lambda t: t.astype(f32).reshape(B_, H, n, CHUNK, t.shape[-1])
    kc, vc = blocks(k), blocks(v)
    g = jnp.cumsum(log_a.astype(f32).reshape(B_, H, n, CHUNK), axis=-1)
    bc = beta.astype(f32).reshape(B_, H, n, CHUNK, 1)
    idx = jnp.arange(CHUNK)
    incl = idx[:, None] >= idx[None, :]
    decay = jnp.where(incl, jnp.exp(jnp.minimum(g[..., :, None] - g[..., None, :], 0.0)), 0.0)
    k_beta = kc * bc
    m = jnp.where(idx[:, None] > idx[None, :], jnp.einsum('bhnik,bhnjk->bhnij', k_beta, kc) * decay, 0.0)
    eye = jnp.eye(CHUNK, dtype=f32)
    p = -m
    t_inv = eye + p
    for _ in range(CHUNK.bit_length() - 2):
        p = p @ p
        t_inv = t_inv @ (eye + p)
    w_blk = jnp.einsum('bhnij,bhnjk->bhnik', t_inv, k_beta * jnp.exp(g)[..., None])
    u_blk = jnp.einsum('bhnij,bhnjv->bhniv', t_inv, vc * bc)
    g_last = g[..., -1:]
    k_tail = kc * jnp.exp(g_last - g)[..., None]
    s_decay = jnp.exp(g_last)[..., None]
    first = lambda t: jnp.moveaxis(t, 2, 0)

    def advance(s, w_n, u_n, kt_n, sd_n):
        u_new = u_n - jnp.einsum('bhik,bhkv->bhiv', w_n, s)
        return s * sd_n + jnp.einsum('bhik,bhiv->bhkv', kt_n, u_new), u_new

    if q is None:
        def state_step(s, xs):
            s_next, _ = advance(s, *xs)
            return s_next, None
        s_fin, _ = lax.scan(state_step, s0, tuple(map(first, (w_blk, u_blk, k_tail, s_decay))))
        return None, s_fin

    qc = blocks(q)
    q_dec = qc * jnp.exp(g)[..., None]
    qk = jnp.einsum('bhnik,bhnjk->bhnij', qc, kc) * decay

    def out_step(s, xs):
        w_n, u_n, kt_n, sd_n, qd_n, qk_n = xs
        s_next, u_new = advance(s, w_n, u_n, kt_n, sd_n)
        o_n = jnp.einsum('bhik,bhkv->bhiv', qd_n, s) + jnp.einsum('bhij,bhjv->bhiv', qk_n, u_new)
        return s_next, o_n

    s_fin, o = lax.scan(out_step, s0, tuple(map(first, (w_blk, u_blk, k_tail, s_decay, q_dec, qk))))
    return jnp.moveaxis(o, 0, 2).reshape(B_, H, L, -1), s_fin


def bidir_delta(q, k, v, log_a, beta, s0):
    rev = lambda t: None if t is None else jnp.flip(t, axis=2)
    o_f, s_f = chunk_gated_delta(q, k, v, log_a[0], beta[0], s0[0])
    o_b, s_b = chunk_gated_delta(rev(q), rev(k), rev(v), rev(log_a[1]), rev(beta[1]), s0[1])
    o = None if q is None else o_f + rev(o_b)
    return o, (s_f, s_b)


def token_mixer(h, w_in, conv_w, a_log, dt_bias, dn_norm, pool_w, pool_scale, w_out, s0, rows):
    B_, L, _ = h.shape
    proj = h @ w_in
    (q, k, v), log_a, beta = deltanet_inputs(proj[..., :QKV_COLS], proj[..., QKV_COLS:STATE_COLS],
                                             conv_w, a_log, dt_bias)
    q = l2norm(q) * DN_HEAD_DIM ** -0.5
    k = l2norm(k)
    o, states = bidir_delta(q, k, v, log_a, beta, s0)
    o = jnp.transpose(o, (0, 2, 1, 3))
    gate = proj[..., STATE_COLS:STATE_COLS + DN_WIDTH].reshape(B_, L, DN_HEADS, DN_HEAD_DIM)
    o = (rmsnorm(o, dn_norm) * jax.nn.silu(gate.astype(jnp.float32))).astype(h.dtype)
    o = o.reshape(B_, L, DN_WIDTH)
    pooled = pool_mixer(proj[..., STATE_COLS + DN_WIDTH:], pool_w, pool_scale, rows)
    return jnp.concatenate([o, pooled], axis=-1) @ w_out, states


def context_states(h, w_in, conv_w, a_log, dt_bias, s0):
    proj = h @ w_in[:, DN_WIDTH:STATE_COLS]
    (k, v), log_a, beta = deltanet_inputs(proj[..., :2 * DN_WIDTH], proj[..., 2 * DN_WIDTH:],
                                          conv_w[:, DN_WIDTH:], a_log, dt_bias)
    _, states = bidir_delta(None, l2norm(k), v, log_a, beta, s0)
    return states


def expert_choice_ffn(h, w_router, w_gate, w_up, w_down):
    B_, L, D = h.shape
    cap = EC_CAPACITY * L // N_EXPERTS
    aff = jax.nn.softmax((h @ w_router).astype(jnp.float32), axis=-1)
    top_aff, top_idx = lax.top_k(jnp.swapaxes(aff, 1, 2), cap)
    xe = jax.vmap(lambda hb, ib: hb[ib])(h, top_idx)
    hid = jax.nn.silu(jnp.einsum('becd,edf->becf', xe, w_gate)) * jnp.einsum('becd,edf->becf', xe, w_up)
    ye = jnp.einsum('becf,efd->becd', hid, w_down) * top_aff[..., None].astype(h.dtype)
    combine = lambda ib, yb: jnp.zeros((L, D), yb.dtype).at[ib.reshape(-1)].add(yb.reshape(-1, D))
    return jax.vmap(combine)(top_idx, ye)


def setup_inputs(seed: int = 0) -> dict:
    key = jax.random.key(seed)
    ks = jax.random.split(key, 21)
    nrm = lambda k, shape, scale: jax.random.normal(k, shape, jnp.float32) * scale
    D = D_MODEL
    dt = jnp.exp(jax.random.uniform(ks[11], (DEPTH, N_DIR, DN_HEADS), jnp.float32,
                                    minval=math.log(1e-3), maxval=math.log(1e-1)))
    return {
        'x': nrm(ks[0], (BATCH, SEQ, D), 1.0),
        'c': nrm(ks[1], (BATCH, D), 1.0),
        'ctx': nrm(ks[2], (BATCH, CTX_LEN, D), 1.0),
        'c_ctx': nrm(ks[3], (D,), 1.0),
        'w_mod': nrm(ks[4], (DEPTH, D, 6 * D), 0.5 * D ** -0.5),
        'b_mod': nrm(ks[5], (DEPTH, 6 * D), 0.01),
        'norm1': 1.0 + nrm(ks[6], (DEPTH, D), 0.02),
        'norm2': 1.0 + nrm(ks[7], (DEPTH, D), 0.02),
        'w_in': nrm(ks[8], (DEPTH, D, IN_COLS), D ** -0.5),
        'conv_w': nrm(ks[9], (DEPTH, CONV_K, QKV_COLS), CONV_K ** -0.5),
        'a_log': jnp.log(jax.random.uniform(ks[10], (DEPTH, N_DIR, DN_HEADS), jnp.float32, minval=1.0, maxval=16.0)),
        'dt_bias': dt + jnp.log(-jnp.expm1(-dt)),
        'dn_norm': 1.0 + nrm(ks[12], (DEPTH, DN_HEAD_DIM), 0.02),
        'pool_w': nrm(ks[13], (DEPTH, POOL_GROUPS, POOL_GROUP_DIM, POOL_GROUP_DIM), POOL_GROUP_DIM ** -0.5),
        'pool_scale': 1.0 + nrm(ks[14], (DEPTH, POOL_WIDTH), 0.1),
        'w_out': nrm(ks[15], (DEPTH, MIX_WIDTH, D), MIX_WIDTH ** -0.5),
        'w_router': nrm(ks[16], (DEPTH, D, N_EXPERTS), D ** -0.5),
        'w_gate': nrm(ks[17], (DEPTH, N_EXPERTS, D, EXPERT_FF), D ** -0.5),
        'w_up': nrm(ks[18], (DEPTH, N_EXPERTS, D, EXPERT_FF), D ** -0.5),
        'w_down': nrm(ks[19], (DEPTH, N_EXPERTS, EXPERT_FF, D), EXPERT_FF ** -0.5),
        'norm_f': 1.0 + nrm(ks[20], (D,), 0.02),
    }


def reference(x, c, ctx, c_ctx, w_mod, b_mod, norm1, norm2, w_in, conv_w, a_log, dt_bias, dn_norm,
              pool_w, pool_scale, w_out, w_router, w_gate, w_up, w_down, norm_f):
    rows = x.shape[1] // GRID_W
    zero = jnp.zeros((ctx.shape[0], DN_HEADS, DN_HEAD_DIM, DN_HEAD_DIM), jnp.float32)
    z = ctx
    for l in range(DEPTH):
        mix_w = (w_in[l], conv_w[l], a_log[l], dt_bias[l], dn_norm[l], pool_w[l], pool_scale[l], w_out[l])
        csh1, csc1, cg1, csh2, csc2, cg2 = adaln_params(c_ctx, w_mod[l], b_mod[l])
        sh1, sc1, g1, sh2, sc2, g2 = [t[:, None, :] for t in adaln_params(c, w_mod[l], b_mod[l])]
        hz = modulate(rmsnorm(z, norm1[l]), csh1, csc1)
        if l < DEPTH - 1:
            yz, ctx_st = token_mixer(hz, *mix_w, (zero, zero), None)
            z = z + cg1 * yz
            hz2 = modulate(rmsnorm(z, norm2[l]), csh2, csc2)
            z = z + cg2 * expert_choice_ffn(hz2, w_router[l], w_gate[l], w_up[l], w_down[l])
        else:
            ctx_st = context_states(hz, w_in[l], conv_w[l], a_log[l], dt_bias[l], (zero, zero))
        h = modulate(rmsnorm(x, norm1[l]), sh1, sc1)
        y, _ = token_mixer(h, *mix_w, ctx_st, rows)
        x = x + g1 * y
        h = modulate(rmsnorm(x, norm2[l]), sh2, sc2)
        x = x + g2 * expert_choice_ffn(h, w_router[l], w_gate[l], w_up[l], w_down[l])
    return rmsnorm(x, norm_f)
```

```python
from contextlib import ExitStack

import numpy as np
import concourse.bass as bass
import concourse.mybir as mybir
from concourse.bass_utils import run_bass_kernel_spmd

F32 = mybir.dt.float32
AF = mybir.ActivationFunctionType
ALU = mybir.AluOpType

D = 1024
KD = D // 128
EPS = 1e-6


class _Op:
    __slots__ = ("eng", "fn", "deps", "signal", "sem", "ticket", "waits", "dma", "idx", "cc", "fence_end")


class Prog:
    STREAMS = ("pe", "act", "dve", "pool", "sp")
    NDMA = {"sp": 12, "act": 4, "pool": 8}

    def __init__(self, nc):
        self.nc = nc
        self.ops = []
        self.last_w = {}
        self.readers = {}

    def add(self, eng, fn, reads=(), writes=(), dma=False, cc=False):
        op = _Op()
        op.eng, op.fn, op.dma, op.signal, op.idx = eng, fn, dma, dma, len(self.ops)
        op.cc = cc
        op.fence_end = False
        deps, raw = set(), set()
        for k in reads:
            w = self.last_w.get(k)
            if w is not None:
                deps.add(w)
                raw.add(w)
        for k in writes:
            w = self.last_w.get(k)
            if w is not None:
                deps.add(w)
            deps.update(self.readers.get(k, ()))
        keep = []
        for d in deps:
            o = self.ops[d]
            same = (not o.dma) and (not dma) and o.eng == eng
            if same and (d not in raw or eng == "pe"):
                continue
            keep.append(d)
        op.deps = sorted(keep)
        for k in writes:
            self.last_w[k] = op.idx
            self.readers[k] = []
        for k in reads:
            if k not in writes:
                self.readers.setdefault(k, []).append(op.idx)
        self.ops.append(op)
        return op

    def fence(self, nopfn, include_cc=False):
        last = {}
        dmas = []
        for o in self.ops[getattr(self, "_fence_at", 0):]:
            if o.cc and not include_cc:
                continue
            if o.dma:
                dmas.append(o.idx)
            elif o.eng != "sp":
                last[o.eng] = o.idx
        deps = sorted(set(last.values()) | set(dmas))
        first = len(self.ops)
        for s in self.STREAMS:
            op = self.add(s, nopfn)
            op.deps = [d for d in deps]
        op.fence_end = True
        self._fence_at = first
        self.last_w = {k: w for k, w in self.last_w.items() if self.ops[w].cc}
        self.readers = {}

    def emit(self, stack):
        nc, ops = self.nc, self.ops
        for op in ops:
            for d in op.deps:
                ops[d].signal = True
        sems = {s: stack.enter_context(nc.semaphore("c_" + s)) for s in ("pe", "act", "dve", "pool")}
        dsems = {s: [stack.enter_context(nc.semaphore("d_%s%d" % (s, i))) for i in range(n)]
                 for s, n in self.NDMA.items()}
        cnt = {s: 0 for s in sems}
        dcnt = {s: 0 for s in dsems}
        known = {s: {} for s in self.STREAMS}
        opknown = [None] * len(ops)
        for op in ops:
            kn = known[op.eng]
            waits = []

            def need(sem, val, src):
                if kn.get(sem, 0) >= val:
                    return
                waits.append((sem, val))
                for k2, v2 in src.items():
                    if kn.get(k2, 0) < v2:
                        kn[k2] = v2

            if op.cc:
                op.sem, op.ticket = stack.enter_context(nc.semaphore("cc%d" % op.idx)), 1
            elif op.dma:
                pool = dsems[op.eng]
                i = dcnt[op.eng]
                dcnt[op.eng] += 1
                sem = pool[i % len(pool)]
                prev = 16 * (i // len(pool))
                if prev:
                    need(sem, prev, {sem: prev})
                op.sem, op.ticket = sem, prev + 16
            for d in op.deps:
                need(ops[d].sem, ops[d].ticket, opknown[d])
            if not op.dma and op.signal:
                cnt[op.eng] += 1
                op.sem, op.ticket = sems[op.eng], cnt[op.eng]
            op.waits = waits
            if op.signal:
                snap = dict(kn)
                snap[op.sem] = max(snap.get(op.sem, 0), op.ticket)
                opknown[op.idx] = snap
            if op.fence_end and max(cnt.values()) > 8000:
                self.nepoch = getattr(self, "nepoch", 0) + 1
                self.maxcnt = max(getattr(self, "maxcnt", 0), max(cnt.values()))
                sems = {s: stack.enter_context(nc.semaphore("c%d_%s" % (self.nepoch, s))) for s in sems}
                cnt = {s: 0 for s in sems}
        self.stats = {"epochs": getattr(self, "nepoch", 0), "maxcnt": getattr(self, "maxcnt", 0), "tickets": dict(cnt), "dma": dict(dcnt), "waits": sum(len(o.waits) for o in ops)}
        by = {s: [o for o in ops if o.eng == s] for s in self.STREAMS}
        block = stack.enter_context(nc.Block())

        def run(eng, lst):
            for op in lst:
                for sem, val in op.waits:
                    eng.wait_ge(sem, val)
                ins = op.fn(eng)
                if op.signal:
                    ins.then_inc(op.sem, 16 if (op.dma and not op.cc) else 1)

        block.tensor(lambda e: run(e, by["pe"]))
        block.scalar(lambda e: run(e, by["act"]))
        block.vector(lambda e: run(e, by["dve"]))
        block.gpsimd(lambda e: run(e, by["pool"]))
        block.sync(lambda e: run(e, by["sp"]))


AX = mybir.AxisListType
NE = 16
QKV = 1536
STATE = 1552
GATE0 = 1552
POOL0 = 2064
INC = 2576
WINS = (2, 4, 8, 16)


class Rot:
    def __init__(self, B, name, n, shape):
        self.t = [(B.sb("%s%d" % (name, i), shape), "%s%d" % (name, i)) for i in range(n)]
        self.i = 0

    def __call__(self):
        r = self.t[self.i % len(self.t)]
        self.i += 1
        return r


class Seq:
    pass


class Builder:
    def __init__(self, L, CTX, depth, FF):
        self.L, self.CTX, self.depth, self.FF = L, CTX, depth, FF
        self.nc = bass.Bass("TRN2", target_bir_lowering=False)
        self.top = ExitStack()
        self.st = self.top
        self.P = Prog(self.nc)

    def dram(self, name, shape, kind="ExternalInput"):
        return self.nc.dram_tensor(name, list(shape), F32, kind=kind).ap()

    def sb(self, name, shape):
        self.uid = getattr(self, "uid", 0) + 1
        return self.st.enter_context(self.nc.sbuf_tensor("s%d_%s" % (self.uid, name), list(shape), F32))

    def dma(self, out, in_, r, w, eng="sp", accum=False):
        if accum:
            self.P.add(eng, lambda e: e.dma_start(out=out, in_=in_, accum_op=ALU.add), reads=r, writes=w, dma=True)
        else:
            self.P.add(eng, lambda e: e.dma_start(out=out, in_=in_), reads=r, writes=w, dma=True)

    def mm(self, out, lhsT, rhs, start, stop, r, w):
        self.P.add("pe", lambda e: e.matmul(out, lhsT=lhsT, rhs=rhs, start=start, stop=stop), reads=r, writes=w)

    def tr(self, out, in_, ident, r, w):
        self.P.add("pe", lambda e: e.transpose(out, in_, ident), reads=r, writes=w)

    def act(self, out, in_, func, r, w, bias=0.0, scale=1.0):
        self.P.add("act", lambda e: e.activation(out=out, in_=in_, func=func, bias=bias, scale=scale), reads=r, writes=w)

    def ts(self, out, in0, s1, s2, op0, op1, r, w, eng="dve"):
        if s2 is None:
            self.P.add(eng, lambda e: e.tensor_scalar(out=out, in0=in0, scalar1=s1, scalar2=None, op0=op0), reads=r, writes=w)
        else:
            self.P.add(eng, lambda e: e.tensor_scalar(out=out, in0=in0, scalar1=s1, scalar2=s2, op0=op0, op1=op1),
                       reads=r, writes=w)

    def tt(self, out, in0, in1, op, r, w, eng="dve"):
        self.P.add(eng, lambda e: e.tensor_tensor(out=out, in0=in0, in1=in1, op=op), reads=r, writes=w)

    def stt(self, out, in0, sc, in1, op0, op1, r, w):
        self.P.add("dve", lambda e: e.scalar_tensor_tensor(out=out, in0=in0, scalar=sc, in1=in1, op0=op0, op1=op1),
                   reads=r, writes=w)

    def cp(self, out, in_, r, w, eng="dve"):
        if eng == "act":
            self.P.add(eng, lambda e: e.copy(out=out, in_=in_), reads=r, writes=w)
        else:
            self.P.add(eng, lambda e: e.tensor_copy(out=out, in_=in_), reads=r, writes=w)

    def rsum(self, out, in_, r, w):
        self.P.add("dve", lambda e: e.reduce_sum(out=out, in_=in_, axis=AX.X), reads=r, writes=w)

    def rmax(self, out, in_, r, w):
        self.P.add("dve", lambda e: e.reduce_max(out=out, in_=in_, axis=AX.X), reads=r, writes=w)

    def recip(self, out, in_, r, w):
        self.P.add("dve", lambda e: e.reciprocal(out=out, in_=in_), reads=r, writes=w)

    def mset(self, out, val, w, eng="pool"):
        self.P.add(eng, lambda e: e.memset(out, val), writes=w)

    def fence(self):
        self.P.fence(lambda e: e.nop())

    def bank(self):
        bs = getattr(self, "bankset", None) or (0, 1, 2, 3, 4, 5)
        self.bcnt = getattr(self, "bcnt", {})
        n_ = self.bcnt.get(bs, 0)
        self.bcnt[bs] = n_ + 1
        i = bs[n_ % len(bs)]
        return self.ps[i], "ps%d" % i

    def setup(self):
        nc = self.nc
        self.ps = [self.st.enter_context(nc.psum_tensor("ps%d" % i, [128, 512], F32)) for i in range(8)]
        self.psn = 0
        self.cst = self.sb("cst", [128, 1024])
        cst = self.dram("cst", [128, 1024])
        self.dma(self.cst[:], cst[:, :], [], ["cst"])
        c = self.cst
        self.ident = c[:, 0:128]
        self.ones = c[:, 128:256]
        self.triF = c[0:64, 256:320]
        self.triB = c[0:64, 320:384]
        self.mask1 = [c[0:64, 384:448], c[0:64, 448:512]]
        self.mask2 = [c[0:64, 576:640], c[0:64, 512:576]]
        self.su128 = c[:, 640:768]
        self.iota = c[:, 768:1024]
        cT = self.dram("cT", [128, KD, 2])
        self.scT = self.sb("scT", [128, KD, 2])
        self.dma(self.scT[:], cT[:, :, :], [], ["scT"])
        self.act(self.scT[:], self.scT[:], AF.Silu, ["scT"], ["scT"])
        self.s0x = self.sb("s0x", [128, 8, 128])
        self.modT = self.sb("modT", [128, 48, 2])
        self.aT = self.sb("aT", [128, 2, KD, 2])
        self.gbc = [[self.sb("gbc%d_%d" % (n, v), [128, D]) for v in range(2)] for n in range(2)]
        self.fence()

    def adaln(self, l):
        I = self.inp
        with ExitStack() as st:
            self.st = st
            modT, aT = self.modT, self.aT
            bm = self.sb("bm", [128, 48])
            nr = self.sb("nr", [128, 2, KD])
            self.dma(bm[:], I["bmodT"][l], [], ["bm"])
            self.dma(nr[:], I["normT"][l], [], ["nr"])
            wv = I["w_mod"][l].rearrange("(j p) n -> p j n", p=128)
            wrot = Rot(self, "wm", 2, [128, KD, 512])
            for cb in range(12):
                wt, wk = wrot()
                self.dma(wt[:], wv[:, :, cb * 512:(cb + 1) * 512], ["w_mod_f%d" % l], [wk])
                ps, pk = self.bank()
                for sub in range(4):
                    for j in range(KD):
                        self.mm(ps[:, sub * 2:sub * 2 + 2], wt[:, j, sub * 128:(sub + 1) * 128], self.scT[:, j, :],
                                j == 0, j == KD - 1, [wk, "scT"], [pk])
                c0 = cb * 4
                self.tt(modT[:, c0:c0 + 4, :], ps[:, 0:8].rearrange("p (a b) -> p a b", b=2),
                        bm[:, c0:c0 + 4].unsqueeze(2).to_broadcast([128, 4, 2]), ALU.add, [pk, "bm"], ["modT"])
            for n in range(2):
                sc = modT[:, (3 * n + 1) * KD:(3 * n + 2) * KD, :]
                self.ts(aT[:, n], sc, 1.0, None, ALU.add, None, ["modT"], ["aT"])
                self.tt(aT[:, n], aT[:, n], nr[:, n, :].unsqueeze(2).to_broadcast([128, KD, 2]), ALU.mult,
                        ["aT", "nr"], ["aT"])
            dg = Rot(self, "dgt", 2, [128, 128])
            for n in range(2):
                for v in range(2):
                    for half in range(2):
                        ps, pk = self.bank()
                        for jj in range(4):
                            j = half * 4 + jj
                            d, dk = dg()
                            self.ts(d[:], self.ident, modT[:, (3 * n + 2) * KD + j, v:v + 1], None, ALU.mult, None,
                                    ["cst", "modT"], [dk])
                            self.mm(ps[:, jj * 128:(jj + 1) * 128], self.ones, d[:], True, True, ["cst", dk], [pk])
                        self.cp(self.gbc[n][v][:, half * 512:(half + 1) * 512], ps[:, 0:512], [pk], ["gbc%d%d" % (n, v)])
            self.fence()
        self.st = self.top

    def norm_tile(self, xt, xk, junk, ssrot):
        ss, sk = ssrot()
        self.act(junk[:], xt, AF.Square, [xk], ["junk"])
        self.rsum(ss[:], junk[:], ["junk"], [sk])
        self.ts(ss[:], ss[:], 1.0 / D, EPS, ALU.mult, ALU.add, [sk], [sk])
        self.act(ss[:], ss[:], AF.Sqrt, [sk], [sk])
        self.recip(ss[:], ss[:], [sk], [sk])
        self.ts(xt, xt, ss[:, 0:1], None, ALU.mult, None, [xk, sk], [xk])

    def hT_tile(self, xn, xk, hT, hk, col0, n, v):
        a = self.aT[:, n]
        sh = self.modT[:, 3 * n * KD:(3 * n + 1) * KD, :]
        for half in range(2):
            ps, pk = self.bank()
            for jj in range(4):
                j = half * 4 + jj
                self.tr(ps[:, jj * 128:(jj + 1) * 128], xn[:, j * 128:(j + 1) * 128], self.ident, [xk, "cst"], [pk])
            for jj in range(4):
                j = half * 4 + jj
                if jj % 2 == 0:
                    self.ts(hT[:, j, col0:col0 + 128], ps[:, jj * 128:(jj + 1) * 128], a[:, j, v:v + 1],
                            sh[:, j, v:v + 1], ALU.mult, ALU.add, [pk, "aT", "modT"], [hk])
                else:
                    self.act(hT[:, j, col0:col0 + 128], ps[:, jj * 128:(jj + 1) * 128], AF.Identity, [pk, "aT", "modT"],
                             [hk], bias=sh[:, j, v:v + 1], scale=a[:, j, v:v + 1])

    def proj_fm(self, wt, wk, hT, hkeys, L, evac):
        blk = min(512, L)
        for tb in range(L // blk):
            ps, pk = self.bank()
            t0 = tb * blk
            for j in range(KD):
                self.mm(ps[:, 0:blk], wt[:, j, :], hT[:, j, t0:t0 + blk], j == 0, j == KD - 1,
                        [wk] + hkeys[t0 // 128:(t0 + blk) // 128], [pk])
            evac(ps, pk, t0, blk)

    def mixer(self, l, S, mode):
        I = self.inp
        L, NT, NC, v = S.L, S.L // 128, S.L // 64, S.v
        full = mode == "full"
        w_in = I["w_in"][l].rearrange("(j p) n -> p j n", p=128)
        with ExitStack() as st:
            self.st = st
            hT = self.sb("hT", [128, KD, L])
            hk = ["hT%d" % i for i in range(NT)]
            junk = self.sb("junk", [128, D])
            ssrot = Rot(self, "ss", 4, [128, 1])
            xrot = Rot(self, "xt", 2, [128, D])
            wrot = Rot(self, "wc", 3, [128, KD, 128])
            for i in range(NT):
                xt, xk = xrot()
                self.dma(xt[:], S.src[i * 128:(i + 1) * 128, :], [S.xk(i)], [xk])
                self.norm_tile(xt[:], xk, junk, ssrot)
                self.hT_tile(xt, xk, hT, hk[i], i * 128, 0, v)
            cv = [self.sb("cv%d" % i, [128, L]) for i in range(3)]
            stB = ExitStack()
            self.st = stB
            wab = self.sb("wab", [128, KD, 16])
            self.dma(wab[:], w_in[:, :, QKV:QKV + 16], ["w_in_f%d" % l], ["wab"])
            gp = self.sb("gp", [64, 16])
            self.dma(gp[:], I["gaB"][l], [], ["gp"])
            ab = self.sb("ab", [64, NC, 16])
            ps, pk = self.bank()
            for c in range(NC):
                for j in range(KD):
                    self.mm(ps[0:64, c * 16:(c + 1) * 16], hT[:, j, c * 64:(c + 1) * 64], wab[:, j, :], j == 0, j == KD - 1,
                            [hk[c // 2], "wab"], [pk])
            self.cp(ab[:], ps[0:64, 0:NC * 16].rearrange("p (c k) -> p c k", k=16), [pk], ["ab"])
            negA = self.sb("negA", [64, 8])
            self.act(negA[:], gp[:, 0:8], AF.Exp, ["gp"], ["negA"])
            self.ts(negA[:], negA[:], -1.0, None, ALU.mult, None, ["negA"], ["negA"])
            la = self.sb("la", [64, NC, 8])
            beta = self.sb("beta", [64, NC, 8])
            self.tt(la[:], ab[:, :, 0:8], gp[:, 8:16].unsqueeze(1).to_broadcast([64, NC, 8]), ALU.add, ["ab", "gp"], ["la"])
            self.act(la[:], la[:], AF.Exp, ["la"], ["la"])
            self.ts(la[:], la[:], 1.0, None, ALU.add, None, ["la"], ["la"])
            self.act(la[:], la[:], AF.Ln, ["la"], ["la"])
            self.tt(la[:], la[:], negA[:].unsqueeze(1).to_broadcast([64, NC, 8]), ALU.mult, ["la", "negA"], ["la"])
            self.act(beta[:], ab[:, :, 8:16], AF.Sigmoid, ["ab"], ["beta"])
            laf = la[:].rearrange("p c k -> p (c k)")
            g = self.sb("g", [64, NC, 8])
            T = self.sb("T", [128, NC, 8])
            ps, pk = self.bank()
            ps2, pk2 = self.bank()
            self.mm(ps[0:64, 0:NC * 8], self.triF, laf, True, True, ["cst", "la"], [pk])
            self.mm(ps[0:64, 256:256 + NC * 8], self.triB, laf, True, True, ["cst", "la"], [pk])
            self.mm(ps2[:, 0:NC * 8], self.ones[0:64, :], laf, True, True, ["cst", "la"], [pk2])
            self.cp(g[:, :, 0:4], ps[0:64, 0:NC * 8].rearrange("p (c k) -> p c k", k=8)[:, :, 0:4], [pk], ["g"])
            self.cp(g[:, :, 4:8], ps[0:64, 256:256 + NC * 8].rearrange("p (c k) -> p c k", k=8)[:, :, 4:8], [pk], ["g"])
            self.cp(T[:], ps2[:, 0:NC * 8].rearrange("p (c k) -> p c k", k=8), [pk2], ["T"])
            eg = self.sb("eg", [64, NC, 8])
            etail = self.sb("etail", [64, NC, 8])
            sd = self.sb("sd", [128, NC, 8])
            bev = self.sb("bev", [64, NC, 8])
            negb = self.sb("negb", [64, NC, 8])
            self.act(eg[:], g[:], AF.Exp, ["g"], ["eg"])
            self.tt(etail[:], T[0:64], g[:], ALU.subtract, ["T", "g"], ["etail"])
            self.act(etail[:], etail[:], AF.Exp, ["etail"], ["etail"])
            self.act(sd[:], T[:], AF.Exp, ["T"], ["sd"])
            self.tt(bev[:], beta[:], eg[:], ALU.mult, ["beta", "eg"], ["bev"])
            self.ts(negb[:], beta[:], -1.0, None, ALU.mult, None, ["beta"], ["negb"])
            gk = ["g", "eg", "etail", "sd", "bev", "negb", "beta"]

            convT = self.sb("convT", [128, 12, 4])
            self.dma(convT[:], I["convT"][l], [], ["convT"])
            dnn = self.sb("dnn", [64, 128])
            self.dma(dnn[:], I["dnB"][l], [], ["dnn"])
            pad = self.sb("pad", [128, L + 3])
            self.mset(pad[:, 0:2], 0.0, ["pad"])
            self.mset(pad[:, L + 2:L + 3], 0.0, ["pad"])
            O = self.sb("O", [64, NC, 128])
            ssts = [self.sb("sst%d" % d_, [128, 128]) for d_ in range(2)]
            RR = []
            for d_ in range(2):
                RR.append({n_: Rot(self, "%s_d%d" % (n_, d_), k_, sh_) for n_, k_, sh_ in (
                    ("qk", 2, [64, 256]), ("Y", 2, [64, 256]), ("kt", 2, [64, 128]), ("qd", 2, [64, 128]),
                    ("fm", 2, [128, 192]), ("jk", 1, [64, 256]), ("s2", 2, [64, 2]), ("dg", 1, [64, 64]),
                    ("tmp", 1, [64, 64]), ("cat", 1, [64, 128]), ("PP", 3, [64, 128]), ("QKm", 2, [64, 64]),
                    ("WT", 2, [128, 64]), ("un", 2, [64, 128]))})
            r_j = Rot(self, "jk", 2, [64, 256])
            r_s2 = Rot(self, "s2", 2, [64, 2])
            r_om = Rot(self, "om", 2, [64, 128])
            r_sg = Rot(self, "sg", 2, [64, 128])
            id64 = self.ident[0:64, 0:64]
            for h in range(4):
                for qi in range(3):
                    if qi == 0 and not full:
                        continue
                    cidx = qi * 4 + h
                    wt, wk = wrot()
                    self.dma(wt[:], w_in[:, :, cidx * 128:(cidx + 1) * 128], ["w_in_f%d" % l], [wk])

                    def ev(ps, pk, t0, blk):
                        self.cp(pad[:, 2 + t0:2 + t0 + blk], ps[:, 0:blk], [pk], ["pad"])
                    self.proj_fm(wt, wk, hT, hk, L, ev)
                    acc = cv[qi]
                    ck = "cv%d" % qi
                    self.ts(acc[:], pad[:, 0:L], convT[:, cidx, 0:1], None, ALU.mult, None, ["pad", "convT"], [ck])
                    for k in range(1, 4):
                        self.stt(acc[:], pad[:, k:k + L], convT[:, cidx, k:k + 1], acc[:], ALU.mult, ALU.add,
                                 ["pad", "convT", ck], [ck])
                    self.act(acc[:], acc[:], AF.Silu, [ck], [ck])
                if full:
                    self.mset(O[:], 0.0, ["O%d" % c_ for c_ in range(NC)], eng="pool")

                def chain(d):
                    R_ = RR[d]
                    r_qk, r_Y, r_kt, r_qd, r_fm, r_j, r_s2 = (R_[n_] for n_ in ("qk", "Y", "kt", "qd", "fm", "jk", "s2"))
                    r_dg, r_tmp, r_cat, r_PP, r_QK, r_WT, r_un = (R_[n_] for n_ in ("dg", "tmp", "cat", "PP", "QKm", "WT", "un"))
                    sst = ssts[d]
                    sstk = "sst%d" % d
                    col = d * 4 + h
                    if S.s0 is None:
                        self.mset(sst[:], 0.0, [sstk], eng="dve")
                    else:
                        self.cp(sst[:], self.s0x[:, col, :], ["s0x"], [sstk])
                    order = range(NC) if d == 0 else range(NC - 1, -1, -1)
                    for c in order:
                        cs = slice(c * 64, (c + 1) * 64)
                        ps, pk = self.bank()
                        for qi in range(3):
                            if qi == 0 and not full:
                                continue
                            self.tr(ps[0:64, qi * 128:(qi + 1) * 128], cv[qi][:, cs], self.ident, ["cv%d" % qi, "cst"], [pk])
                            yield
                        lo = 0 if full else 128
                        jk, jkk = r_j()
                        s2, s2k = r_s2()
                        self.act(jk[:, lo:256], ps[0:64, lo:256], AF.Square, [pk], [jkk])
                        self.rsum(s2[:, lo // 128:2], jk[:, lo:256].rearrange("p (a b) -> p a b", b=128), [jkk], [s2k])
                        sl = s2[:, lo // 128:2]
                        self.ts(sl, sl, EPS, None, ALU.add, None, [s2k], [s2k])
                        self.act(sl, sl, AF.Sqrt, [s2k], [s2k])
                        self.recip(sl, sl, [s2k], [s2k])
                        yield
                        qk, qkk = r_qk()
                        if full:
                            self.ts(qk[:, 0:128], ps[0:64, 0:128], s2[:, 0:1], float(128 ** -0.5), ALU.mult, ALU.mult,
                                    [pk, s2k], [qkk])
                        self.ts(qk[:, 128:256], ps[0:64, 128:256], s2[:, 1:2], None, ALU.mult, None, [pk, s2k], [qkk])
                        Y, Yk = r_Y()
                        self.ts(Y[:, 0:128], qk[:, 128:256], bev[:, c, col:col + 1], None, ALU.mult, None, [qkk, "bev"], [Yk])
                        self.ts(Y[:, 128:256], ps[0:64, 256:384], beta[:, c, col:col + 1], None, ALU.mult, None,
                                [pk, "beta"], [Yk])
                        kt, ktk = r_kt()
                        self.ts(kt[:], qk[:, 128:256], etail[:, c, col:col + 1], None, ALU.mult, None, [qkk, "etail"], [ktk])
                        yield
                        fm, fmk = r_fm()
                        ps2, pk2 = self.bank()
                        self.tr(ps2[:, 0:64], qk[:, 128:256], id64, [qkk, "cst"], [pk2])
                        if full:
                            qd, qdk = r_qd()
                            self.ts(qd[:], qk[:, 0:128], eg[:, c, col:col + 1], None, ALU.mult, None, [qkk, "eg"], [qdk])
                            self.tr(ps2[:, 64:128], qk[:, 0:128], id64, [qkk, "cst"], [pk2])
                            self.tr(ps2[:, 128:192], qd[:], id64, [qdk, "cst"], [pk2])
                            self.cp(fm[:], ps2[:, 0:192], [pk2], [fmk])
                        else:
                            self.cp(fm[:, 0:64], ps2[:, 0:64], [pk2], [fmk])
                            yield
                        ps3, pk3 = self.bank()
                        self.mm(ps3[0:64, 0:64], fm[:, 0:64], fm[:, 0:64], True, True, [fmk], [pk3])
                        if full:
                            self.mm(ps3[0:64, 64:128], fm[:, 0:64], fm[:, 64:128], True, True, [fmk], [pk3])
                        dg, dgk = r_dg()
                        self.ts(dg[:], id64, g[:, c, col:col + 1], None, ALU.mult, None, ["cst", "g"], [dgk])
                        self.mm(ps3[0:64, 128:192], self.ones[0:64, 0:64], dg[:], True, True, ["cst", dgk], [pk3])
                        yield
                        tmp, tmk = r_tmp()
                        self.ts(tmp[:], ps3[0:64, 128:192], g[:, c, col:col + 1], None, ALU.subtract, None, [pk3, "g"], [tmk])
                        cat, catk = r_cat()
                        self.ts(cat[:, 0:64], tmp[:], 0.0, -1.0, ALU.max, ALU.mult, [tmk], [catk])
                        self.ts(cat[:, 64:128], tmp[:], 0.0, None, ALU.min, None, [tmk], [catk])
                        self.act(cat[:], cat[:], AF.Exp, [catk], [catk])
                        yield
                        PP, PPk = r_PP()
                        self.stt(PP[:, 0:64], cat[:, 0:64], negb[:, c, col:col + 1], ps3[0:64, 0:64], ALU.mult, ALU.mult,
                                 [catk, "negb", pk3], [PPk])
                        self.tt(PP[:, 0:64], PP[:, 0:64], self.mask1[d], ALU.mult, [PPk, "cst"], [PPk])
                        yield
                        if full:
                            QK, QKk = r_QK()
                            self.tt(QK[:], cat[:, 64:128], ps3[0:64, 64:128], ALU.mult, [catk, pk3], [QKk])
                            self.tt(QK[:], QK[:], self.mask2[d], ALU.mult, [QKk, "cst"], [QKk])
                        ps4, pk4 = self.bank()
                        self.tr(ps4[0:64, 0:64], PP[:, 0:64], id64, [PPk, "cst"], [pk4])
                        self.cp(PP[:, 64:128], ps4[0:64, 0:64], [pk4], [PPk])
                        yield
                        for s_ in range(6):
                            psY, pkY = self.bank()
                            self.mm(psY[0:64, 0:256], PP[:, 64:128], Y[:], True, True, [PPk, Yk], [pkY])
                            self.tt(Y[:], Y[:], psY[0:64, 0:256], ALU.add, [Yk, pkY], [Yk])
                            yield
                            if s_ < 5:
                                psP, pkP = self.bank()
                                self.mm(psP[0:64, 0:64], PP[:, 64:128], PP[:, 0:64], True, True, [PPk], [pkP])
                                self.mm(psP[0:64, 64:128], PP[:, 0:64], PP[:, 64:128], True, True, [PPk], [pkP])
                                PP, PPk = r_PP()
                                self.cp(PP[:], psP[0:64, 0:128], [pkP], [PPk], eng="act")
                                yield
                        WT, WTk = r_WT()
                        psW, pkW = self.bank()
                        self.tr(psW[:, 0:64], Y[:, 0:128], id64, [Yk, "cst"], [pkW])
                        self.cp(WT[:], psW[:, 0:64], [pkW], [WTk], eng="act")
                        yield
                        psU, pkU = self.bank()
                        self.mm(psU[0:64, 0:128], WT[:], sst[:], True, True, [WTk, sstk], [pkU])
                        un, unk = r_un()
                        self.tt(un[:], Y[:, 128:256], psU[0:64, 0:128], ALU.subtract, [Yk, pkU], [unk])
                        yield
                        if full:
                            psO, pkO = self.bank()
                            self.mm(psO[0:64, 0:128], fm[:, 128:192], sst[:], True, False, [fmk, sstk], [pkO])
                            self.mm(psO[0:64, 0:128], QK[:], un[:], False, True, [QKk, unk], [pkO])
                            self.tt(O[:, c, :], O[:, c, :], psO[0:64, 0:128], ALU.add, ["O%d" % c, pkO], ["O%d" % c])
                            yield
                        psS, pkS = self.bank()
                        self.mm(psS[:, 0:128], kt[:], un[:], True, True, [ktk, unk], [pkS])
                        self.stt(sst[:], sst[:], sd[:, c, col:col + 1], psS[:, 0:128], ALU.mult, ALU.add,
                                 [sstk, "sd", pkS], [sstk])
                    if S.sfin:
                        self.cp(self.s0x[:, col, :], sst[:], [sstk], ["s0x"])

                gens = [(chain(0), (0, 1, 2)), (chain(1), (3, 4, 5))]
                while gens:
                    for gb_ in list(gens):
                        self.bankset = gb_[1]
                        try:
                            next(gb_[0])
                        except StopIteration:
                            gens.remove(gb_)
                self.bankset = None
                if full:
                    wt, wk = wrot()
                    self.dma(wt[:], w_in[:, :, GATE0 + h * 128:GATE0 + (h + 1) * 128], ["w_in_f%d" % l], [wk])
                    for c in range(NC):
                        psG, pkG = self.bank()
                        for j in range(KD):
                            self.mm(psG[0:64, 0:128], hT[:, j, c * 64:(c + 1) * 64], wt[:, j, :], j == 0, j == KD - 1,
                                    [hk[c // 2], wk], [pkG])
                        sg, sgk = r_sg()
                        self.act(sg[:], psG[0:64, 0:128], AF.Silu, [pkG], [sgk])
                        jk, jkk = r_j()
                        s2, s2k = r_s2()
                        self.act(jk[:, 0:128], O[:, c, :], AF.Square, ["O%d" % c], [jkk])
                        self.rsum(s2[:, 0:1], jk[:, 0:128], [jkk], [s2k])
                        self.ts(s2[:, 0:1], s2[:, 0:1], 1.0 / 128, EPS, ALU.mult, ALU.add, [s2k], [s2k])
                        self.act(s2[:, 0:1], s2[:, 0:1], AF.Sqrt, [s2k], [s2k])
                        self.recip(s2[:, 0:1], s2[:, 0:1], [s2k], [s2k])
                        om, omk = r_om()
                        self.stt(om[:], O[:, c, :], s2[:, 0:1], dnn[:], ALU.mult, ALU.mult, ["O%d" % c, s2k, "dnn"], [omk])
                        self.tt(om[:], om[:], sg[:], ALU.mult, [omk, sgk], [omk])
                        self.dma(S.mix[c * 64:(c + 1) * 64, h * 128:(h + 1) * 128], om[:], [omk], ["mix%d_%d" % (c, h)])
            self.fence()
            stB.close()
            stC = ExitStack()
            self.st = stC
            if full:
                R, W = S.R, S.W
                pw = self.sb("pw", [128, 4, 128])
                self.dma(pw[:], I["pool_w"][l].rearrange("g c d -> c g d"), [], ["pw"])
                psc = self.sb("psc", [128, 512])
                self.dma(psc[:], I["pscB"][l], [], ["psc"])
                icn = self.sb("icn", [128, L])
                r_y = Rot(self, "yp", 2, [128, 128])
                U, A_, B_ = cv[0], cv[1], cv[2]
                v3 = (lambda t: t[:].rearrange("p (r w) -> p r w", w=W))
                for gi in range(4):
                    w_ = WINS[gi]
                    a_ = w_ // 2
                    wt, wk = wrot()
                    self.dma(wt[:], w_in[:, :, POOL0 + gi * 128:POOL0 + (gi + 1) * 128], ["w_in_f%d" % l], [wk])
                    self.dma(icn[:], S.icnt[gi], [], ["icn"])

                    def ev(ps, pk, t0, blk):
                        self.cp(U[:, t0:t0 + blk], ps[:, 0:blk], [pk], ["cv0"])
                    self.proj_fm(wt, wk, hT, hk, L, ev)
                    self.cp(A_[:], U[:], ["cv0"], ["cv1"])
                    for off in range(-a_, a_):
                        if off == 0 or abs(off) >= W:
                            continue
                        if off > 0:
                            o_, i_ = v3(A_)[:, :, 0:W - off], v3(U)[:, :, off:W]
                        else:
                            o_, i_ = v3(A_)[:, :, -off:W], v3(U)[:, :, 0:W + off]
                        self.tt(o_, o_, i_, ALU.add, ["cv1", "cv0"], ["cv1"])
                    src_ = A_
                    sk_ = "cv1"
                    if R > 1:
                        self.cp(B_[:], A_[:], ["cv1"], ["cv2"])
                        for off in range(-a_, a_):
                            if off == 0 or abs(off) >= R:
                                continue
                            if off > 0:
                                o_, i_ = v3(B_)[:, 0:R - off, :], v3(A_)[:, off:R, :]
                            else:
                                o_, i_ = v3(B_)[:, -off:R, :], v3(A_)[:, 0:R + off, :]
                            self.tt(o_, o_, i_, ALU.add, ["cv2", "cv1"], ["cv2"])
                        src_, sk_ = B_, "cv2"
                    self.tt(src_[:], src_[:], icn[:], ALU.mult, [sk_, "icn"], [sk_])
                    self.tt(src_[:], src_[:], U[:], ALU.subtract, [sk_, "cv0"], [sk_])
                    for i in range(NT):
                        psy, pky = self.bank()
                        self.mm(psy[:, 0:128], src_[:, i * 128:(i + 1) * 128], pw[:, gi, :], True, True, [sk_, "pw"], [pky])
                        y, yk = r_y()
                        self.tt(y[:], psy[:, 0:128], psc[:, gi * 128:(gi + 1) * 128], ALU.mult, [pky, "psc"], [yk])
                        self.dma(S.mix[i * 128:(i + 1) * 128, 512 + gi * 128:512 + (gi + 1) * 128], y[:], [yk],
                                 ["mixp%d_%d" % (i, gi)])
                wo = self.sb("wo", [128, KD, D])
                self.dma(wo[:], I["w_out"][l].rearrange("(j p) n -> p j n", p=128), ["w_out_f%d" % l], ["wo"])
                mT = Rot(self, "mT", 2, [128, KD, 128])
                gb = self.gbc[0][v]
                gbk = "gbc0%d" % v
                for i in range(NT):
                    mt, mk = xrot()
                    mkeys = ["mix%d_%d" % (c, h) for c in (2 * i, 2 * i + 1) for h in range(4)] + \
                            ["mixp%d_%d" % (i, gi) for gi in range(4)]
                    self.dma(mt[:], S.mix[i * 128:(i + 1) * 128, :], mkeys, [mk])
                    m, mTk = mT()
                    for half in range(2):
                        ps, pk = self.bank()
                        for jj in range(4):
                            j = half * 4 + jj
                            self.tr(ps[:, jj * 128:(jj + 1) * 128], mt[:, j * 128:(j + 1) * 128], self.ident, [mk, "cst"], [pk])
                        self.cp(m[:, half * 4:(half + 1) * 4, :], ps[:, 0:512].rearrange("p (a b) -> p a b", b=128), [pk], [mTk],
                                eng="act" if half else "dve")
                    xt, xk = xrot()
                    self.dma(xt[:], S.src[i * 128:(i + 1) * 128, :], [S.xk(i)], [xk])
                    for nb in range(2):
                        psy, pky = self.bank()
                        for j in range(KD):
                            self.mm(psy[:, 0:512], m[:, j, :], wo[:, j, nb * 512:(nb + 1) * 512], j == 0, j == KD - 1,
                                    [mTk, "wo"], [pky])
                        self.tt(junk[:, nb * 512:(nb + 1) * 512], psy[:, 0:512], gb[:, nb * 512:(nb + 1) * 512], ALU.mult,
                                [pky, gbk], ["junk"])
                    self.tt(xt[:], xt[:], junk[:], ALU.add, [xk, "junk"], [xk])
                    self.dma(S.dst[i * 128:(i + 1) * 128, :], xt[:], [xk], [S.dk(i)])
            self.fence()
            stC.close()
        self.st = self.top

    def ffn(self, l, S):
        I = self.inp
        L, NT, v, FF = S.L, S.L // 128, S.v, self.FF
        FFC = FF // 128
        cap = 2 * L // NE
        NCC = (cap + 127) // 128
        with ExitStack() as st:
            self.st = st
            xn = self.sb("xn", [128, NT, D])
            mk_ = self.sb("mk", [128, NT, NE])
            sel = self.sb("sel", [128, NT, NE])
            stT = ExitStack()
            self.st = stT
            junk = self.sb("junk", [128, D])
            ssrot = Rot(self, "ss", 4, [128, 1])
            h2 = Rot(self, "h2T", 2, [128, KD, 128])
            wr = self.sb("wr", [128, KD, NE])
            self.dma(wr[:], I["w_router"][l].rearrange("(j p) n -> p j n", p=128), [], ["wr"])
            psl, pkl = self.ps[6], "ps6"
            for i in range(NT):
                self.dma(xn[:, i, :], S.dst[i * 128:(i + 1) * 128, :], [S.dk(i)], ["xn%d" % i])
                self.norm_tile(xn[:, i, :], "xn%d" % i, junk, ssrot)
                ht, htk = h2()
                self.hT_tile(xn[:, i, :], "xn%d" % i, ht, htk, 0, 1, v)
                for j in range(KD):
                    self.mm(psl[:, i * NE:(i + 1) * NE], ht[:, j, :], wr[:, j, :], j == 0, j == KD - 1, [htk, "wr"], [pkl])
            aff = self.sb("aff", [128, NT, NE])
            mx = self.sb("mx", [128, NT])
            self.cp(aff[:], psl[:, 0:NT * NE].rearrange("p (i e) -> p i e", e=NE), [pkl], ["aff"])
            self.rmax(mx[:], aff[:], ["aff"], ["mx"])
            self.tt(aff[:], aff[:], mx[:].unsqueeze(2).to_broadcast([128, NT, NE]), ALU.subtract, ["aff", "mx"], ["aff"])
            self.act(aff[:], aff[:], AF.Exp, ["aff"], ["aff"])
            self.rsum(mx[:], aff[:], ["aff"], ["mx"])
            self.recip(mx[:], mx[:], ["mx"], ["mx"])
            self.tt(aff[:], aff[:], mx[:].unsqueeze(2).to_broadcast([128, NT, NE]), ALU.mult, ["aff", "mx"], ["aff"])
            affT = self.sb("affT", [NE, L])
            work = self.sb("work", [NE, L])
            for i0 in range(0, NT, 4):
                ps, pk = self.bank()
                n_ = min(4, NT - i0)
                for ii in range(n_):
                    self.tr(ps[0:NE, ii * 128:(ii + 1) * 128], aff[:, i0 + ii, :], self.ident, ["aff", "cst"], [pk])
                self.cp(affT[:, i0 * 128:(i0 + n_) * 128], ps[0:NE, 0:n_ * 128], [pk], ["affT"])
            self.cp(work[:], affT[:], ["affT"], ["work"])
            m8 = self.sb("m8", [NE, 8])
            for r_ in range(cap // 8):
                self.P.add("dve", lambda e: e.max(out=m8[:], in_=work[:]), reads=["work"], writes=["m8"])
                self.P.add("dve", lambda e: e.match_replace(out=work[:], in_to_replace=m8[:], in_values=work[:], imm_value=0.0),
                           reads=["work", "m8"], writes=["work"])
            self.tt(work[:], affT[:], work[:], ALU.subtract, ["affT", "work"], ["work"])
            self.ts(work[:], work[:], 0.0, None, ALU.is_gt, None, ["work"], ["work"])
            ps, pk = self.bank()
            for i in range(NT):
                self.tr(ps[:, i * NE:(i + 1) * NE], work[:, i * 128:(i + 1) * 128], self.ident[0:NE, 0:NE], ["work", "cst"], [pk])
            self.cp(mk_[:], ps[:, 0:NT * NE].rearrange("p (i e) -> p i e", e=NE), [pk], ["mk"])
            self.tt(sel[:], aff[:], mk_[:], ALU.mult, ["aff", "mk"], ["sel"])
            self.fence()
            stT.close()
            self.st = st
            mkf = mk_[:].rearrange("p i e -> p (i e)")
            rk = self.sb("rk", [128, NT, NE])
            tot = self.sb("tot", [128, NT, NE])
            ps, pk = self.bank()
            self.mm(ps[:, 0:NT * NE], self.su128, mkf, True, True, ["cst", "mk"], [pk])
            self.mm(ps[:, 256:256 + NT * NE], self.ones, mkf, True, True, ["cst", "mk"], [pk])
            self.cp(rk[:], ps[:, 0:NT * NE].rearrange("p (i e) -> p i e", e=NE), [pk], ["rk"])
            self.cp(tot[:], ps[:, 256:256 + NT * NE].rearrange("p (i e) -> p i e", e=NE), [pk], ["tot"])
            off = self.sb("off", [128, NT, NE])
            self.mset(off[:, 0, :], 0.0, ["off"], eng="dve")
            for i in range(1, NT):
                self.tt(off[:, i, :], off[:, i - 1, :], tot[:, i - 1, :], ALU.add, ["off", "tot"], ["off"])
            self.tt(rk[:], rk[:], off[:], ALU.add, ["rk", "off"], ["rk"])
            self.ts(rk[:], rk[:], 1.0, None, ALU.add, None, ["rk"], ["rk"])
            self.tt(rk[:], rk[:], mk_[:], ALU.mult, ["rk", "mk"], ["rk"])
            self.ts(rk[:], rk[:], -1.0, None, ALU.add, None, ["rk"], ["rk"])
            Srot = Rot(self, "S", 1, [128, NT, cap])
            STrot = Rot(self, "ST", 1, [128, NCC, L])
            xe_r = Rot(self, "xeT", 1, [128, KD, cap])
            hid_r = Rot(self, "hid", 1, [128, FFC, cap])
            ye_r = Rot(self, "ye", 1, [128, NCC, D])
            ac_r = Rot(self, "affc", 2, [128, NCC])
            sg_r = Rot(self, "sgt", 2, [128, cap])
            ct_r = Rot(self, "ct", 2, [128, D])
            wg_r = Rot(self, "wg", 2, [128, KD, 128])
            wu_r = Rot(self, "wu", 2, [128, KD, 128])
            wd_r = Rot(self, "wd", 1 if FFC > 4 else 2, [128, FFC, 512])
            a2 = self.aT[:, 1]
            sh2 = self.modT[:, 3 * KD:4 * KD, :]
            gb = self.gbc[1][v]
            gbk = "gbc1%d" % v
            for e_ in range(NE):
                wgv = I["w_gate"][l][e_].rearrange("(j p) n -> p j n", p=128)
                wuv = I["w_up"][l][e_].rearrange("(j p) n -> p j n", p=128)
                wdv = I["w_down"][l][e_].rearrange("(f p) n -> p f n", p=128)
                Se, Sk = Srot()
                for i in range(NT):
                    self.ts(Se[:, i, :], self.iota[:, 0:cap], rk[:, i, e_:e_ + 1], None, ALU.is_equal, None,
                            ["cst", "rk"], [Sk], eng="dve" if i % 2 == 0 else "pool")
                xe, xek = xe_r()
                for j in range(KD):
                    ps, pk = self.bank()
                    for i in range(NT):
                        self.mm(ps[:, 0:cap], xn[:, i, j * 128:(j + 1) * 128], Se[:, i, :], i == 0, i == NT - 1,
                                ["xn%d" % i, Sk], [pk])
                    self.ts(xe[:, j, :], ps[:, 0:cap], a2[:, j, v:v + 1], sh2[:, j, v:v + 1], ALU.mult, ALU.add,
                            [pk, "aT", "modT"], [xek])
                ac, ack = ac_r()
                ps, pk = self.bank()
                for cc in range(NCC):
                    ccn = min(128, cap - cc * 128)
                    for i in range(NT):
                        self.mm(ps[0:ccn, cc:cc + 1], Se[:, i, cc * 128:cc * 128 + ccn], sel[:, i, e_:e_ + 1], i == 0, i == NT - 1,
                                [Sk, "sel"], [pk])
                    self.cp(ac[0:ccn, cc:cc + 1], ps[0:ccn, cc:cc + 1], [pk], [ack])
                hid, hidk = hid_r()
                for fc in range(FFC):
                    wg, wgk = wg_r()
                    wu, wuk = wu_r()
                    self.dma(wg[:], wgv[:, :, fc * 128:(fc + 1) * 128], ["w_gate_f%d" % l], [wgk])
                    self.dma(wu[:], wuv[:, :, fc * 128:(fc + 1) * 128], ["w_up_f%d" % l], [wuk])
                    psg, pkg = self.bank()
                    psu, pku = self.bank()
                    for j in range(KD):
                        self.mm(psg[:, 0:cap], wg[:, j, :], xe[:, j, :], j == 0, j == KD - 1, [wgk, xek], [pkg])
                    for j in range(KD):
                        self.mm(psu[:, 0:cap], wu[:, j, :], xe[:, j, :], j == 0, j == KD - 1, [wuk, xek], [pku])
                    sg, sgk = sg_r()
                    self.act(sg[:], psg[:, 0:cap], AF.Silu, [pkg], [sgk])
                    self.tt(hid[:, fc, :], sg[:], psu[:, 0:cap], ALU.mult, [sgk, pku], [hidk])
                ye, yek = ye_r()
                for nb in range(2):
                    wd, wdk = wd_r()
                    self.dma(wd[:], wdv[:, :, nb * 512:(nb + 1) * 512], ["w_down_f%d" % l], [wdk])
                    for cc in range(NCC):
                        ccn = min(128, cap - cc * 128)
                        ps, pk = self.bank()
                        for fc in range(FFC):
                            self.mm(ps[0:ccn, 0:512], hid[:, fc, cc * 128:cc * 128 + ccn], wd[:, fc, :],
                                    fc == 0, fc == FFC - 1, [hidk, wdk], [pk])
                        self.ts(ye[0:ccn, cc, nb * 512:(nb + 1) * 512], ps[0:ccn, 0:512], ac[0:ccn, cc:cc + 1], None, ALU.mult, None,
                                [pk, ack], [yek])
                ST, STk = STrot()
                for cc in range(NCC):
                    ccn = min(128, cap - cc * 128)
                    for i0 in range(0, NT, 4):
                        ps, pk = self.bank()
                        n_ = min(4, NT - i0)
                        for ii in range(n_):
                            self.tr(ps[0:ccn, ii * 128:(ii + 1) * 128], Se[:, i0 + ii, cc * 128:cc * 128 + ccn], self.ident,
                                    [Sk, "cst"], [pk])
                        self.cp(ST[0:ccn, cc, i0 * 128:(i0 + n_) * 128], ps[0:ccn, 0:n_ * 128], [pk], [STk],
                                eng="act" if (i0 // 4) % 2 else "dve")
                for i in range(NT):
                    ct, ctk = ct_r()
                    for nb in range(2):
                        ps, pk = self.bank()
                        for cc in range(NCC):
                            ccn = min(128, cap - cc * 128)
                            self.mm(ps[:, 0:512], ST[0:ccn, cc, i * 128:(i + 1) * 128], ye[0:ccn, cc, nb * 512:(nb + 1) * 512],
                                    cc == 0, cc == NCC - 1, [STk, yek], [pk])
                        self.tt(ct[:, nb * 512:(nb + 1) * 512], ps[:, 0:512], gb[:, nb * 512:(nb + 1) * 512], ALU.mult,
                                [pk, gbk], [ctk])
                    self.dma(S.dst[i * 128:(i + 1) * 128, :], ct[:], [ctk, S.dk(i)], [S.dk(i)], eng="pool", accum=True)
            self.fence()
        self.st = self.top

    def final_norm(self, S, out):
        I = self.inp
        with ExitStack() as st:
            self.st = st
            nf = self.sb("nf", [128, D])
            self.dma(nf[:], I["nfB"], [], ["nf"])
            junk = self.sb("junk", [128, D])
            ssrot = Rot(self, "ss", 4, [128, 1])
            xrot = Rot(self, "xt", 2, [128, D])
            keys = []
            for i in range(S.L // 128):
                xt, xk = xrot()
                self.dma(xt[:], S.dst[i * 128:(i + 1) * 128, :], [S.dk(i)], [xk])
                self.norm_tile(xt[:], xk, junk, ssrot)
                self.tt(xt[:], xt[:], nf[:], ALU.mult, [xk, "nf"], [xk])
                self.dma(out[i * 128:(i + 1) * 128, :], xt[:], [xk], ["out%d" % i])
                keys.append("out%d" % i)
            self.P.add("sp", lambda e: e.nop(), reads=keys)
        self.st = self.top


NCORES = 8


def _big_specs(FF):
    return [("w_mod", D, 6 * D), ("w_in", D, INC), ("w_out", D, D),
            ("w_gate", NE * D, FF), ("w_up", NE * D, FF), ("w_down", NE * FF, D)]


def build(L, CTX, depth, FF, gather=False):
    B = Builder(L, CTX, depth, FF)
    B.setup()
    d = B.dram
    I = {
        "x": d("x", [L, D]), "ctx": d("ctx", [CTX, D]),
        "bmodT": d("bmodT", [depth, 128, 48]),
        "normT": d("normT", [depth, 128, 2, KD]),
        "convT": d("convT", [depth, 128, 12, 4]), "gaB": d("gaB", [depth, 64, 16]),
        "dnB": d("dnB", [depth, 64, 128]), "pool_w": d("pool_w", [depth, 4, 128, 128]),
        "pscB": d("pscB", [depth, 128, 512]),
        "w_router": d("w_router", [depth, D, NE]),
        "nfB": d("nfB", [128, D]), "icx": d("icx", [4, 128, L]), "icc": d("icc", [4, 128, CTX]),
    }
    big = {}
    for name, rows, cols in _big_specs(FF):
        if not gather:
            t = d(name, [depth, rows, cols])
            big[name] = [t[l] for l in range(depth)]
            continue
        rs = rows // NCORES
        sh = d(name + "_sh", [depth, rs, cols])
        big[name] = []
        for l in range(depth):
            bounce = d("%s_b%d" % (name, l), [rs, cols], "Internal")
            full = d("%s_f%d" % (name, l), [rows, cols], "Internal")
            per = rs // 128 * cols
            shv = sh[l].rearrange("(p a) c -> p (a c)", p=128)
            bv = bounce.rearrange("(p a) c -> p (a c)", p=128)
            keys = []
            for o in range(0, per, 8192):
                n_ = min(8192, per - o)
                B.dma(bv[:, o:o + n_], shv[:, o:o + n_], [], ["%s_b%d_%d" % (name, l, o)], eng="pool")
                keys.append("%s_b%d_%d" % (name, l, o))

            def ag(bounce=bounce, full=full, keys=keys, name=name, l=l):
                B.P.add("pool", lambda e: e.collective_compute("AllGather", ALU.bypass,
                                                               replica_groups=[list(range(NCORES))],
                                                               ins=[bounce.opt()], outs=[full.opt()]),
                        reads=keys, writes=["%s_f%d" % (name, l)], dma=True, cc=True)
            ag()
            big[name].append(full)
    if gather:
        B.P.fence(lambda e: e.nop(), include_cc=True)
    I["w_mod"] = big["w_mod"]
    I["w_in"] = big["w_in"]
    I["w_out"] = big["w_out"]
    I["w_gate"] = [t.rearrange("(e k) n -> e k n", e=NE) for t in big["w_gate"]]
    I["w_up"] = [t.rearrange("(e k) n -> e k n", e=NE) for t in big["w_up"]]
    I["w_down"] = [t.rearrange("(e k) n -> e k n", e=NE) for t in big["w_down"]]
    B.inp = I
    out = d("out", [L, D], "ExternalOutput")
    xs = d("xs", [L, D], "Internal")
    zs = d("zs", [CTX, D], "Internal")
    mixx = d("mixx", [L, D], "Internal")
    mixc = d("mixc", [CTX, D], "Internal")
    for l in range(depth):
        last = l == depth - 1
        B.adaln(l)
        Sc = Seq()
        Sc.L, Sc.v, Sc.R, Sc.W, Sc.icnt = CTX, 1, 1, CTX, I["icc"]
        Sc.src = I["ctx"] if l == 0 else zs
        Sc.dst, Sc.mix, Sc.s0, Sc.sfin = zs, mixc, None, True
        Sc.xk = (lambda i: "zs%d" % i)
        Sc.dk = (lambda i: "zs%d" % i)
        B.mixer(l, Sc, "states" if last else "full")
        if not last:
            B.ffn(l, Sc)
        Sx = Seq()
        Sx.L, Sx.v, Sx.R, Sx.W, Sx.icnt = L, 0, L // 64, 64, I["icx"]
        Sx.src = I["x"] if l == 0 else xs
        Sx.dst, Sx.mix, Sx.s0, Sx.sfin = xs, mixx, True, False
        Sx.xk = (lambda i: "xs%d" % i)
        Sx.dk = (lambda i: "xs%d" % i)
        B.mixer(l, Sx, "full")
        B.ffn(l, Sx)
    B.final_norm(Sx, out)
    B.P.emit(B.top)
    B.top.close()
    return B


def _box_icnt(n_rows, n_cols):
    res = []
    for w in WINS:
        def cnt(n):
            pos = np.arange(n)
            return np.clip(pos - w // 2 + w, 0, n) - np.clip(pos - w // 2, 0, n)
        c = (cnt(n_rows)[:, None] * cnt(n_cols)[None, :]).astype(np.float32)
        res.append((1.0 / c).reshape(-1))
    return np.stack(res).astype(np.float32)


def _consts():
    c = np.zeros((128, 1024), np.float32)
    c[:, 0:128] = np.eye(128)
    c[:, 128:256] = 1.0
    p = np.arange(64)[:, None]
    f = np.arange(64)[None, :]
    c[0:64, 256:320] = (p <= f)
    c[0:64, 320:384] = (p >= f)
    c[0:64, 384:448] = (p > f)
    c[0:64, 448:512] = (f > p)
    c[0:64, 512:576] = (f <= p)
    c[0:64, 576:640] = (f >= p)
    P = np.arange(128)[:, None]
    Fq = np.arange(128)[None, :]
    c[:, 640:768] = (P < Fq)
    c[:, 768:1024] = np.arange(256)[None, :]
    return c


def layout_inputs(b, inp, L, CTX, depth, FF, gather=False):
    f = lambda a: np.ascontiguousarray(np.asarray(a, dtype=np.float32))
    bigd = {}
    for name, rows, cols in _big_specs(FF):
        w3 = np.asarray(inp[name], dtype=np.float32).reshape(depth, rows, cols)
        if gather:
            rs = rows // NCORES
            bigd[name + "_sh"] = f(w3[:, b * rs:(b + 1) * rs])
        else:
            bigd[name] = f(w3)
    cT = np.stack([inp["c"][b], inp["c_ctx"]], -1).reshape(KD, 128, 2).transpose(1, 0, 2)
    normT = np.stack([inp["norm1"], inp["norm2"]], 1).reshape(depth, 2, KD, 128).transpose(0, 3, 1, 2)
    gaB = np.concatenate([inp["a_log"].reshape(depth, 8), inp["dt_bias"].reshape(depth, 8)], -1)
    return {
        **bigd,
        "cst": _consts(), "cT": f(cT), "x": f(inp["x"][b]), "ctx": f(inp["ctx"][b]),
        "bmodT": f(inp["b_mod"].reshape(depth, 48, 128).transpose(0, 2, 1)),
        "normT": f(normT),
        "convT": f(inp["conv_w"].reshape(depth, 4, 12, 128).transpose(0, 3, 2, 1)),
        "gaB": f(np.broadcast_to(gaB[:, None, :], (depth, 64, 16))),
        "dnB": f(np.broadcast_to(inp["dn_norm"][:, None, :], (depth, 64, 128))),
        "pool_w": f(inp["pool_w"]), "pscB": f(np.broadcast_to(inp["pool_scale"][:, None, :], (depth, 128, 512))),
        "w_router": f(inp["w_router"]), "nfB": f(np.broadcast_to(inp["norm_f"][None, :], (128, D))),
        "icx": f(np.broadcast_to(_box_icnt(L // 64, 64)[:, None, :], (4, 128, L))),
        "icc": f(np.broadcast_to(_box_icnt(1, CTX)[:, None, :], (4, 128, CTX))),
    }


def run_module(inputs, L, CTX, depth, FF):
    prog = build(L, CTX, depth, FF, gather=True)
    in_maps = [layout_inputs(b, inputs, L, CTX, depth, FF, gather=True) for b in range(NCORES)]
    res = run_bass_kernel_spmd(prog.nc, in_maps, core_ids=list(range(NCORES)))
    return np.stack([np.asarray(r["out"], dtype=np.float32) for r in res.results], 0)


def kernel(**inputs):
    inputs = {k: np.asarray(v) for k, v in inputs.items()}
    return run_module(inputs, 2048, 256, 4, 1024)
```
